# Optimizing a Trainium2 kernel written in Bass

```python
import math
import jax
import jax.numpy as jnp
from jax import lax
import numpy as np

D_MODEL = 1024
BATCH = 2
SEQ = 16384
DEPTH = 2

SSD_EXPAND = 1
SSD_INNER = SSD_EXPAND * D_MODEL
SSD_HEAD_DIM = 64
SSD_HEADS = SSD_INNER // SSD_HEAD_DIM
SSD_GROUPS = 4
SSD_HEADS_PER_GROUP = SSD_HEADS // SSD_GROUPS
SSD_STATE = 128
SSD_CONV = 4
SSD_CHUNK = 128
SSD_CONV_DIM = SSD_INNER + 2 * SSD_GROUPS * SSD_STATE

SB_HEADS = 4
SB_HEAD_DIM = 128
SB_WIDTH = SB_HEADS * SB_HEAD_DIM
SB_BLOCK = 128
SB_SUB = 32

POOL_WINDOWS = (2, 4, 8, 16)
POOL_GROUPS = 4
POOL_WIDTH = D_MODEL // 2
POOL_GROUP_DIM = POOL_WIDTH // POOL_GROUPS

N_BRANCHES = 3

FFN_DENSE = 2816
N_EXPERTS = 8
TOP_K = 2
FFN_EXPERT = 1792
MOE_BLOCK = 512

EPS = 1e-6

COL_Z = SSD_INNER
COL_XBC = COL_Z + SSD_CONV_DIM
COL_DT = COL_XBC + SSD_HEADS
COL_Q = COL_DT + SB_WIDTH
COL_K = COL_Q + SB_WIDTH
COL_V = COL_K + SB_WIDTH
COL_POOL = COL_V + POOL_WIDTH
IN_WIDTH = COL_POOL + N_BRANCHES * D_MODEL

kernel_name = "hybrid_ssd_stickbreak_pool_moe"


def _rms(xf):
    return xf * lax.rsqrt(jnp.mean(xf * xf, axis=-1, keepdims=True) + EPS)


def rms_norm(x, g):
    y = _rms(x.astype(jnp.float32)) * g.astype(jnp.float32)
    return y.astype(x.dtype)


def causal_depthwise_conv(u, w, b):
    ch = u.shape[-1]
    out = lax.conv_general_dilated(
        u, w[:, None, :], window_strides=(1,), padding=[(SSD_CONV - 1, 0)],
        dimension_numbers=("NWC", "WIO", "NWC"), feature_group_count=ch)
    return out + b


def ssd_branch(z, xbc, dt_raw, conv_w, conv_b, dt_bias, a_log, d_skip, norm_g):
    f32 = jnp.float32
    bsz, seq, _ = z.shape
    nc, L = seq // SSD_CHUNK, SSD_CHUNK
    G, R, P, N = SSD_GROUPS, SSD_HEADS_PER_GROUP, SSD_HEAD_DIM, SSD_STATE
    xbc = jax.nn.silu(causal_depthwise_conv(xbc.astype(f32), conv_w.astype(f32), conv_b.astype(f32)))
    xs, b_in, c_in = jnp.split(xbc, [SSD_INNER, SSD_INNER + G * N], axis=-1)
    dt = jax.nn.softplus(dt_raw.astype(f32) + dt_bias.astype(f32))
    da = dt * (-jnp.exp(a_log.astype(f32)))
    x_dt = (xs.reshape(bsz, seq, SSD_HEADS, P) * dt[..., None]).reshape(bsz, nc, L, G, R, P)
    bm = b_in.reshape(bsz, nc, L, G, N)
    cm = c_in.reshape(bsz, nc, L, G, N)
    a_cs = jnp.cumsum(da.reshape(bsz, nc, L, G, R).transpose(0, 1, 3, 4, 2), axis=-1)

    causal = jnp.tril(jnp.ones((L, L), dtype=bool))
    seg = a_cs[..., :, None] - a_cs[..., None, :]
    decay = jnp.exp(jnp.where(causal, seg, -jnp.inf))
    cb = jnp.einsum("bclgn,bcsgn->bcgls", cm, bm)
    y_diag = jnp.einsum("bcgrls,bcsgrp->bclgrp", cb[:, :, :, None] * decay, x_dt)

    def chunk_step(state, inp):
        b_c, c_c, x_c, acs_c = inp
        y_off = jnp.einsum("blgn,bgrpn,bgrl->blgrp", c_c, state, jnp.exp(acs_c))
        to_end = jnp.exp(acs_c[..., -1:] - acs_c)
        new_state = (state * jnp.exp(acs_c[..., -1])[..., None, None]
                     + jnp.einsum("blgn,bgrl,blgrp->bgrpn", b_c, to_end, x_c))
        return new_state, y_off

    state0 = jnp.zeros((bsz, G, R, P, N), f32)
    _, y_off = lax.scan(chunk_step, state0,
                        (jnp.moveaxis(bm, 1, 0), jnp.moveaxis(cm, 1, 0),
                         jnp.moveaxis(x_dt, 1, 0), jnp.moveaxis(a_cs, 1, 0)))
    y = (y_diag + jnp.moveaxis(y_off, 0, 1)).reshape(bsz, seq, SSD_HEADS, P)
    y = y + xs.reshape(bsz, seq, SSD_HEADS, P) * d_skip.astype(f32)[:, None]
    y = y.reshape(bsz, seq, SSD_INNER) * jax.nn.silu(z.astype(f32))
    y = _rms(y.reshape(bsz, seq, G, SSD_INNER // G)).reshape(bsz, seq, SSD_INNER)
    return y * norm_g.astype(f32)


def stick_breaking_attention(q, k, v, q_norm_g, k_norm_g):
    f32 = jnp.float32
    bsz, seq, _ = q.shape
    H, Dh = SB_HEADS, SB_HEAD_DIM
    scale = 1.0 / math.sqrt(Dh)
    qh = (rms_norm(q.reshape(bsz, seq, H, Dh).astype(f32), q_norm_g) * scale).transpose(0, 2, 1, 3)
    kh = rms_norm(k.reshape(bsz, seq, H, Dh).astype(f32), k_norm_g).transpose(0, 2, 1, 3)
    vh = v.reshape(bsz, seq, H, Dh).astype(f32).transpose(0, 2, 1, 3)
    sub_ids = jnp.arange(SB_SUB)
    tri_sub = (sub_ids[:, None] > sub_ids[None, :]).astype(f32)
    q_local = jnp.arange(SB_BLOCK)
    outs = []
    for i in range(seq // SB_BLOCK):
        lk = (i + 1) * SB_BLOCK
        nk = lk // SB_SUB
        z = jnp.einsum("bhqd,bhkd->bhqk", qh[:, :, i * SB_BLOCK:lk], kh[:, :, :lk])
        mask = jnp.arange(lk)[None, :] < (i * SB_BLOCK + q_local)[:, None]
        log_beta = jnp.minimum(z, 0.0) - jnp.log1p(jnp.exp(-jnp.abs(z)))
        log_rest = jnp.where(mask, log_beta - z, 0.0)
        log_rest = log_rest.reshape(bsz, H, SB_BLOCK, nk, SB_SUB)
        local = jnp.einsum("bhqkj,js->bhqks", log_rest, tri_sub)
        blk_ids = jnp.arange(nk)
        tri_blk = (blk_ids[:, None] > blk_ids[None, :]).astype(f32)
        later = jnp.einsum("bhqk,kl->bhql", jnp.sum(log_rest, axis=-1), tri_blk)
        suffix = (local + later[..., None]).reshape(bsz, H, SB_BLOCK, lk)
        weights = jnp.where(mask, jnp.exp(log_beta + suffix), 0.0)
        outs.append(jnp.einsum("bhqk,bhkd->bhqd", weights, vh[:, :, :lk]))
    out = jnp.concatenate(outs, axis=2)
    return out.transpose(0, 2, 1, 3).reshape(bsz, seq, SB_WIDTH)


def multiscale_pool(u, pool_w, pool_scale):
    f32 = jnp.float32
    bsz, seq, _ = u.shape
    grp = u.astype(f32).reshape(bsz, seq, POOL_GROUPS, POOL_GROUP_DIM)
    csum = jnp.cumsum(grp, axis=1)
    t = jnp.arange(seq)
    outs = []
    for gi, win in enumerate(POOL_WINDOWS):
        c = csum[:, :, gi]
        prev = jnp.pad(c, ((0, 0), (win, 0), (0, 0)))[:, :seq]
        count = jnp.minimum(t + 1, win).astype(f32)[None, :, None]
        outs.append((c - prev) / count - grp[:, :, gi])
    pooled = jnp.stack(outs, axis=2)
    mixed = jnp.einsum("bsgi,gio->bsgo", pooled, pool_w.astype(f32))
    return mixed.reshape(bsz, seq, POOL_WIDTH) * pool_scale.astype(f32)


def hybrid_mixer(h, w_in, conv_w, conv_b, dt_bias, a_log, d_skip, ssd_norm_g, q_norm_g, k_norm_g,
                 pool_w, pool_scale, w_br_ssd, w_br_sb, w_br_pool, w_out):
    bsz, seq, _ = h.shape
    proj = h @ w_in
    z, xbc, dt_raw, q, k, v, u, gates = jnp.split(
        proj, [COL_Z, COL_XBC, COL_DT, COL_Q, COL_K, COL_V, COL_POOL], axis=-1)
    y_ssd = ssd_branch(z, xbc, dt_raw, conv_w, conv_b, dt_bias, a_log, d_skip, ssd_norm_g).astype(h.dtype)
    y_sb = stick_breaking_attention(q, k, v, q_norm_g, k_norm_g).astype(h.dtype)
    y_pool = multiscale_pool(u, pool_w, pool_scale).astype(h.dtype)
    g = jax.nn.sigmoid(gates.astype(jnp.float32)).reshape(bsz, seq, N_BRANCHES, D_MODEL)
    merged = (g[:, :, 0] * (y_ssd @ w_br_ssd).astype(jnp.float32)
              + g[:, :, 1] * (y_sb @ w_br_sb).astype(jnp.float32)
              + g[:, :, 2] * (y_pool @ w_br_pool).astype(jnp.float32))
    return merged.astype(h.dtype) @ w_out


def swiglu(h, w_gate, w_up, w_down):
    return (jax.nn.silu(h @ w_gate) * (h @ w_up)) @ w_down


def moe_swiglu(h, router_w, e_gate, e_up, e_down):
    f32 = jnp.float32
    bsz, seq, d = h.shape
    tokens = h.reshape(-1, d)
    n_tok = tokens.shape[0]
    logits = tokens.astype(f32) @ router_w.astype(f32)
    top_val, top_idx = lax.top_k(logits, TOP_K)
    top_w = jax.nn.softmax(top_val, axis=-1)
    n_assign = n_tok * TOP_K
    cap = -(-n_assign // MOE_BLOCK) * MOE_BLOCK + N_EXPERTS * MOE_BLOCK
    n_blocks = cap // MOE_BLOCK
    expert_flat = top_idx.reshape(-1).astype(jnp.int32)
    weight_flat = top_w.reshape(-1)
    token_flat = jnp.arange(n_assign, dtype=jnp.int32) // TOP_K
    order = jnp.argsort(expert_flat)
    sorted_exp = expert_flat[order]
    counts = jnp.zeros((N_EXPERTS,), jnp.int32).at[expert_flat].add(1)
    padded = (counts + MOE_BLOCK - 1) // MOE_BLOCK * MOE_BLOCK
    starts = jnp.cumsum(counts) - counts
    pends = jnp.cumsum(padded)
    pstarts = pends - padded
    dest = pstarts[sorted_exp] + jnp.arange(n_assign, dtype=jnp.int32) - starts[sorted_exp]
    slot_tok = jnp.full((cap,), n_tok, jnp.int32).at[dest].set(token_flat[order])
    slot_w = jnp.zeros((cap,), f32).at[dest].set(weight_flat[order])
    block_exp = jnp.minimum(jnp.searchsorted(pends, jnp.arange(n_blocks) * MOE_BLOCK, side="right"),
                            N_EXPERTS - 1).astype(jnp.int32)
    x_pad = jnp.concatenate([tokens, jnp.zeros((1, d), tokens.dtype)], axis=0)
    xb = x_pad[slot_tok].reshape(n_blocks, MOE_BLOCK, d)

    def expert_block(args):
        xblk, e = args
        return swiglu(xblk, e_gate[e], e_up[e], e_down[e])

    yb = lax.map(expert_block, (xb, block_exp)).reshape(cap, d)
    out = jnp.zeros((n_tok + 1, d), f32).at[slot_tok].add(yb.astype(f32) * slot_w[:, None])
    return out[:n_tok].astype(h.dtype).reshape(bsz, seq, d)


def setup_inputs(seed: int = 0) -> dict:
    key = jax.random.key(seed)
    ks = jax.random.split(key, 32)
    n_dense = (DEPTH + 1) // 2
    n_moe = DEPTH // 2

    def nrm(k, shape, fan_in):
        return jax.random.normal(k, shape, jnp.float32) * (fan_in ** -0.5)

    def gain(k, shape):
        return 1.0 + 0.02 * jax.random.normal(k, shape, jnp.float32)

    dt0 = jnp.exp(jax.random.uniform(ks[5], (DEPTH, SSD_HEADS), jnp.float32,
                                     minval=math.log(1e-3), maxval=math.log(1e-1)))
    dt0 = jnp.maximum(dt0, 1e-4)
    dt_bias = dt0 + jnp.log(-jnp.expm1(-dt0))
    a_log = jnp.log(jax.random.uniform(ks[6], (DEPTH, SSD_HEADS), jnp.float32, minval=1.0, maxval=16.0))
    return {
        "x": jax.random.normal(ks[0], (BATCH, SEQ, D_MODEL), jnp.float32),
        "mix_norm_g": gain(ks[1], (DEPTH, D_MODEL)),
        "w_in": nrm(ks[2], (DEPTH, D_MODEL, IN_WIDTH), D_MODEL),
        "conv_w": nrm(ks[3], (DEPTH, SSD_CONV, SSD_CONV_DIM), SSD_CONV),
        "conv_b": 0.01 * jax.random.normal(ks[4], (DEPTH, SSD_CONV_DIM), jnp.float32),
        "dt_bias": dt_bias,
        "a_log": a_log,
        "d_skip": gain(ks[7], (DEPTH, SSD_HEADS)),
        "ssd_norm_g": gain(ks[8], (DEPTH, SSD_INNER)),
        "q_norm_g": gain(ks[9], (DEPTH, SB_HEAD_DIM)),
        "k_norm_g": gain(ks[10], (DEPTH, SB_HEAD_DIM)),
        "pool_w": nrm(ks[11], (DEPTH, POOL_GROUPS, POOL_GROUP_DIM, POOL_GROUP_DIM), POOL_GROUP_DIM),
        "pool_scale": gain(ks[12], (DEPTH, POOL_WIDTH)),
        "w_br_ssd": nrm(ks[13], (DEPTH, SSD_INNER, D_MODEL), SSD_INNER),
        "w_br_sb": nrm(ks[14], (DEPTH, SB_WIDTH, D_MODEL), SB_WIDTH),
        "w_br_pool": nrm(ks[15], (DEPTH, POOL_WIDTH, D_MODEL), POOL_WIDTH),
        "w_out": nrm(ks[16], (DEPTH, D_MODEL, D_MODEL), D_MODEL),
        "ffn_norm_g": gain(ks[17], (DEPTH, D_MODEL)),
        "ffn_w_gate": nrm(ks[18], (n_dense, D_MODEL, FFN_DENSE), D_MODEL),
        "ffn_w_up": nrm(ks[19], (n_dense, D_MODEL, FFN_DENSE), D_MODEL),
        "ffn_w_down": nrm(ks[20], (n_dense, FFN_DENSE, D_MODEL), FFN_DENSE),
        "router_w": nrm(ks[21], (n_moe, D_MODEL, N_EXPERTS), D_MODEL),
        "moe_w_gate": nrm(ks[22], (n_moe, N_EXPERTS, D_MODEL, FFN_EXPERT), D_MODEL),
        "moe_w_up": nrm(ks[23], (n_moe, N_EXPERTS, D_MODEL, FFN_EXPERT), D_MODEL),
        "moe_w_down": nrm(ks[24], (n_moe, N_EXPERTS, FFN_EXPERT, D_MODEL), FFN_EXPERT),
    }


def reference(x, mix_norm_g, w_in, conv_w, conv_b, dt_bias, a_log, d_skip, ssd_norm_g, q_norm_g,
              k_norm_g, pool_w, pool_scale, w_br_ssd, w_br_sb, w_br_pool, w_out, ffn_norm_g,
              ffn_w_gate, ffn_w_up, ffn_w_down, router_w, moe_w_gate, moe_w_up, moe_w_down):
    for layer in range(DEPTH):
        h = rms_norm(x, mix_norm_g[layer])
        x = x + hybrid_mixer(h, w_in[layer], conv_w[layer], conv_b[layer], dt_bias[layer], a_log[layer],
                             d_skip[layer], ssd_norm_g[layer], q_norm_g[layer], k_norm_g[layer],
                             pool_w[layer], pool_scale[layer], w_br_ssd[layer], w_br_sb[layer],
                             w_br_pool[layer], w_out[layer])
        h = rms_norm(x, ffn_norm_g[layer])
        i = layer // 2
        if layer % 2 == 0:
            x = x + swiglu(h, ffn_w_gate[i], ffn_w_up[i], ffn_w_down[i])
        else:
            x = x + moe_swiglu(h, router_w[i], moe_w_gate[i], moe_w_up[i], moe_w_down[i])
    return x
```

```python
from contextlib import ExitStack
import os
import math
import numpy as np
import ml_dtypes
import concourse.bass as bass
import concourse.mybir as mybir
from concourse.bass_utils import run_bass_kernel_spmd

F32 = mybir.dt.float32
BF16 = mybir.dt.bfloat16
AF = mybir.ActivationFunctionType
ALU = mybir.AluOpType
AX = mybir.AxisListType

COMPUTE = ("pe", "act", "dve", "pool")
N_DMA_SEMS = 12
EPS = 1e-6


class Prog:
    def __init__(self, nc, stack):
        self.nc = nc
        self.stack = stack
        self.streams = {k: [] for k in COMPUTE + ("sp",)}
        self.cnt = {}
        self.sem = {}
        for k in COMPUTE:
            self.sem[k] = stack.enter_context(nc.semaphore("s_" + k))
            self.cnt[k] = 0
        for i in range(N_DMA_SEMS):
            k = "d%d" % i
            self.sem[k] = stack.enter_context(nc.semaphore("s_" + k))
            self.cnt[k] = 0
        self.dma_rr = 0
        self.last_w = {}
        self.readers = {}
        self.seen = {k: {} for k in COMPUTE + ("sp",)}
        self.n_ops = 0

    def sb(self, name, shape, dt=F32):
        return self.stack.enter_context(self.nc.sbuf_tensor(name, list(shape), dt))

    def ps(self, name, shape, dt=F32):
        return self.stack.enter_context(self.nc.psum_tensor(name, list(shape), dt))

    def _deps(self, eng, reads, writes):
        need = {}

        def add(src, idx):
            if idx > need.get(src, 0):
                need[src] = idx

        for k in reads:
            w = self.last_w.get(k)
            if w is not None:
                add(*w)
        for k in writes:
            w = self.last_w.get(k)
            if w is not None:
                add(*w)
            for src, idx in self.readers.get(k, {}).items():
                if src != eng:
                    add(src, idx)
        out = []
        seen = self.seen[eng]
        for src, idx in need.items():
            if seen.get(src, 0) >= idx:
                continue
            seen[src] = idx
            out.append((src, idx))
        return out

    def _commit(self, src, idx, reads, writes):
        for k in reads:
            self.readers.setdefault(k, {})[src] = idx
        for k in writes:
            self.last_w[k] = (src, idx)
            self.readers[k] = {}

    def op(self, eng, meth, kw, reads=(), writes=()):
        waits = self._deps(eng, reads, writes)
        self.cnt[eng] += 1
        idx = self.cnt[eng]
        self._commit(eng, idx, reads, writes)
        sem = self.sem
        self.n_ops += 1

        def run(e):
            for src, v in waits:
                e.wait_ge(sem[src], v)
            getattr(e, meth)(**kw).then_inc(sem[eng], 1)

        self.streams[eng].append(run)

    def dma(self, out, in_, reads=(), writes=(), queue="sp"):
        d = "d%d" % self.dma_rr
        self.dma_rr = (self.dma_rr + 1) % N_DMA_SEMS
        waits = self._deps(queue, reads, writes)
        prev = self.cnt[d]
        if prev > 0 and self.seen[queue].get(d, 0) < prev:
            self.seen[queue][d] = prev
            waits.append((d, prev))
        self.cnt[d] = prev + 16
        idx = self.cnt[d]
        self._commit(d, idx, reads, writes)
        sem = self.sem
        self.n_ops += 1

        def run(e):
            for src, v in waits:
                e.wait_ge(sem[src], v)
            e.dma_start(out=out, in_=in_).then_inc(sem[d], 16)

        self.streams[queue].append(run)

    def finish(self):
        nc = self.nc
        sem, cnt = self.sem, self.cnt
        streams = self.streams
        with nc.Block() as block:
            @block.tensor
            def _(e):
                for f in streams["pe"]:
                    f(e)

            @block.scalar
            def _(e):
                for f in streams["act"]:
                    f(e)

            @block.vector
            def _(e):
                for f in streams["dve"]:
                    f(e)

            @block.gpsimd
            def _(e):
                for f in streams["pool"]:
                    f(e)

            @block.sync
            def _(e):
                for f in streams["sp"]:
                    f(e)
                for i in range(N_DMA_SEMS):
                    k = "d%d" % i
                    if cnt[k] > 0:
                        e.wait_ge(sem[k], cnt[k])
                for k in COMPUTE:
                    if cnt[k] > 0:
                        e.wait_ge(sem[k], cnt[k])


CONST_NAMES = ("ident", "LE", "GT", "LT", "negGE", "ones", "Pfirst", "Pmain", "Pprev")
NCONST = len(CONST_NAMES)
CA_XS, CA_B, CA_C, CA_Q, CA_K, CA_Z, CA_V, CA_U, CA_DT, CA_END = 0, 256, 384, 512, 640, 768, 1024, 1152, 1280, 1344
RW_DTB, RW_ALOG, RW_DSK, RW_NG, RW_END = 0, 4, 8, 264, 520


def host_consts(window):
    p = np.arange(128)[:, None]
    f = np.arange(128)[None, :]
    c = {}
    c["ident"] = (p == f)
    c["LE"] = (p <= f)
    c["GT"] = (p > f)
    c["LT"] = (p < f)
    c["negGE"] = -1.0 * (p >= f)
    c["ones"] = np.ones((128, 128))
    w = window
    band = ((f - p) >= 0) & ((f - p) < w)
    c["Pmain"] = band / float(w) - (p == f)
    cnt = np.minimum(f + 1, w).astype(np.float64)
    c["Pfirst"] = band / cnt - (p == f)
    bandp = ((f + 128 - p) >= 0) & ((f + 128 - p) < w)
    c["Pprev"] = bandp / float(w)
    return np.concatenate([np.asarray(c[n], np.float32) for n in CONST_NAMES], axis=1)


def build_A(S):
    NT = S // 512
    NB = S // 128
    nc = bass.Bass("TRN2", target_bir_lowering=False)

    def din(name, shape, dt=F32):
        return nc.dram_tensor(name, list(shape), dt, kind="ExternalInput").ap()

    xT = din("xT", [128, 8, S])
    wA = din("wA", [128, 8, CA_END])
    gmix = din("gmix", [128, 8])
    convw = din("convw", [128, 16])
    convb = din("convb", [128, 4])
    rows = din("rows", [1, RW_END])
    qkg = din("qkg", [128, 2])
    poolw = din("poolw", [128, 128])
    pscale = din("pscale", [128, 1])
    consts = din("consts", [128, NCONST * 128])
    yT = nc.dram_tensor("yT", [4, 128, S], BF16, kind="ExternalOutput").ap()

    with ExitStack() as st:
        P = Prog(nc, st)
        sb, ps = P.sb, P.ps
        cst = sb("cst", [128, NCONST, 128])
        cstb = sb("cstb", [128, NCONST, 128], BF16)
        C32 = {n: cst[:, i, :] for i, n in enumerate(CONST_NAMES)}
        CBF = {n: cstb[:, i, :] for i, n in enumerate(CONST_NAMES)}
        wst = sb("wst", [128, 8, 128])
        wbf = sb("wbf", [128, 8, CA_END], BF16)
        gmx = sb("gmx", [128, 8])
        cw = sb("cw", [128, 16]); cb = sb("cb", [128, 4])
        rowb = sb("rowb", [128, RW_END])
        negA = sb("negA", [128, 4])
        qk_g = sb("qk_g", [128, 2])
        pw32 = sb("pw32", [128, 128]); pwb = sb("pwb", [128, 128], BF16)
        psc = sb("psc", [128, 1])
        zerob = sb("zerob", [128, 128], BF16)
        KT = sb("KT", [128, S], BF16)
        VA = sb("VA", [128, NB, 128], BF16)
        xt0_ = sb("xt0", [128, 8, 512]); xt = [xt0_, xt0_]
        sq = sb("sq", [128, 8, 512], BF16)
        rstd = sb("rstd", [128, 512]); lnv = rstd
        hT = sb("hT", [128, 8, 512], BF16)
        ubuf = sb("ubuf", [128, 4, 515])
        cacc = sb("cacc", [128, 512])
        xbcT = sb("xbcT", [128, 4, 512], BF16)
        qraw = sb("qraw", [128, 512]); qsq = sb("qsq", [128, 512], BF16)
        qrs = sb("qrs", [128, 512]); qln = qrs
        qn = sb("qn", [128, 512], BF16)
        siluz = sb("siluz", [128, 4, 256])
        utok = sb("utok", [128, 5, 128])
        dt256 = sb("dt256", [128, 256]); da64 = sb("da64", [128, 64]); dtt = sb("dtt", [128, 16]); dte = sb("dte", [128, 16]); dtv = sb("dtv", [128, 16]); da = da64[:, 0:16]
        T48 = sb("T48", [128, 48]); E48 = sb("E48", [128, 48])
        U = sb("U", [128, 4, 128])
        eseg = sb("eseg", [128, 4, 128])
        cbm = sb("cbm", [128, 128])
        MT = sb("MT", [128, 4, 128], BF16)
        xtok = sb("xtok", [128, 256]); btok = sb("btok", [128, 128], BF16)
        dtte = sb("dtte", [128, 4])
        xdt = sb("xdt", [128, 4, 64], BF16); xw = sb("xw", [128, 4, 64], BF16)
        stT = sb("stT", [128, 256]); stb = sb("stb", [128, 256], BF16)
        y1 = sb("y1", [128, 256]); y2 = sb("y2", [128, 256]); ysq = y2
        ss = sb("ss", [128, 1]); ssl = sb("ssl", [128, 1]); srs = sb("srs", [128, 1])
        yo = sb("yo", [128, 256], BF16)
        plT = sb("plT", [128, 512], BF16)
        ystage = [sb("ystage0", [128, 4, 512], BF16), sb("ystage1", [128, 4, 512], BF16)]
        ex = [sb("ex0", [128, 512]), sb("ex1", [128, 512])]
        spb = [sb("spb0", [128, 512], BF16), sb("spb1", [128, 512], BF16)]
        arg = [sb("arg0", [128, 512]), sb("arg1", [128, 512])]
        Wb = [sb("Wb0", [128, 512], BF16), sb("Wb1", [128, 512], BF16)]
        Rb = sb("Rb", [128, 512])
        psA = [ps("psA0", [128, 512]), ps("psA1", [128, 512])]
        psB = [ps("psB0", [128, 512]), ps("psB1", [128, 512])]
        psC = ps("psC", [128, 512])
        psO = ps("psO", [128, 512])
        psM = [ps("psM0", [128, 512]), ps("psM1", [128, 512])]
        mrr = [0]

        def nextM():
            i = mrr[0]
            mrr[0] ^= 1
            return psM[i], "psM%d" % i

        P.dma(cst[:].rearrange("p a b -> p (a b)"), consts, writes=["cst"])
        P.op("dve", "tensor_copy", dict(out=cstb[:], in_=cst[:]), ["cst"], ["cstb"])
        P.dma(gmx[:], gmix, writes=["gmx"])
        P.dma(cw[:], convw, writes=["cw"])
        P.dma(cb[:], convb, writes=["cb"])
        P.dma(rowb[:], rows.partition_broadcast(128), writes=["rowb"])
        P.dma(qk_g[:], qkg, writes=["qk_g"])
        P.dma(pw32[:], poolw, writes=["pw32"])
        P.dma(psc[:], pscale, writes=["psc"])
        P.op("dve", "tensor_copy", dict(out=pwb[:], in_=pw32[:]), ["pw32"], ["pwb"])
        P.op("dve", "tensor_scalar", dict(out=qk_g[:, 0:1], in0=qk_g[:, 0:1], scalar1=1.0 / math.sqrt(128.0),
                                              scalar2=None, op0=ALU.mult), ["qk_g"], ["qk_g"])
        P.op("act", "activation", dict(out=negA[:], in_=rowb[:, RW_ALOG:RW_ALOG + 4], func=AF.Exp), ["rowb"], ["negA"])
        P.op("dve", "tensor_scalar", dict(out=negA[:], in0=negA[:], scalar1=-1.0, scalar2=None, op0=ALU.mult),
             ["negA"], ["negA"])
        P.op("pool", "memset", dict(ap=zerob[:], constant=0.0), [], ["zerob"])
        P.op("pool", "memset", dict(ap=ubuf[:], constant=0.0), [], ["ubuf"])
        P.op("pool", "memset", dict(ap=stT[:], constant=0.0), [], ["stT"])
        P.op("pool", "memset", dict(ap=stb[:], constant=0.0), [], ["stb"])
        P.op("pool", "memset", dict(ap=utok[:], constant=0.0), [], ["utok"])
        P.op("pool", "memset", dict(ap=da64[:], constant=0.0), [], ["da"])
        c0 = 0
        wi = 0
        while c0 < CA_END:
            c1 = min(c0 + 128, CA_END)
            n = c1 - c0
            P.dma(wst[:, :, 0:n], wA[:, :, c0:c1], writes=["wst"])
            eng = "act" if wi % 2 == 0 else "dve"
            if eng == "act":
                P.op("act", "activation", dict(out=wbf[:, :, c0:c1], in_=wst[:, :, 0:n], func=AF.Copy),
                     ["wst"], ["wbf"])
            else:
                P.op("dve", "tensor_copy", dict(out=wbf[:, :, c0:c1], in_=wst[:, :, 0:n]),
                     ["wst"], ["wbf"])
            c0 = c1
            wi += 1

        def mm(out, lhsT, rhs, start, stop, reads, writes):
            P.op("pe", "matmul", dict(out=out, lhsT=lhsT, rhs=rhs, start=start, stop=stop), reads, writes)

        def front(I):
            t0 = 512 * I
            s = I % 2
            X = xt[s]; Xk = "xt0"
            P.dma(X[:], xT[:, :, t0:t0 + 512], writes=[Xk])
            P.op("act", "activation", dict(out=sq[:], in_=X[:], func=AF.Square), [Xk], ["sq"])
            pm, pk = nextM()
            for kc in range(8):
                mm(pm[:], CBF["ones"], sq[:, kc, :], kc == 0, kc == 7, ["cstb", "sq"], [pk])
            P.op("act", "activation", dict(out=lnv[:], in_=pm[:], func=AF.Ln, scale=1.0 / 1024.0, bias=EPS), [pk], ["rstd"])
            P.op("act", "activation", dict(out=rstd[:], in_=lnv[:], func=AF.Exp, scale=-0.5), ["rstd"], ["rstd"])
            for kc in range(8):
                P.op("dve", "scalar_tensor_tensor", dict(out=hT[:, kc, :], in0=X[:, kc, :], scalar=gmx[:, kc:kc + 1],
                                                                     in1=rstd[:], op0=ALU.mult, op1=ALU.mult),
                     [Xk, "gmx", "rstd"], ["hT"])
            FS = int(os.environ.get("FS", "9"))
            if FS < 2:
                return
            for c in range(6 if FS >= 3 else 4):
                pm, pk = nextM()
                for kc in range(8):
                    mm(pm[:], wbf[:, kc, 128 * c:128 * (c + 1)], hT[:, kc, :], kc == 0, kc == 7, ["wbf", "hT"], [pk])
                if c < 4:
                    P.op("act", "activation", dict(out=ubuf[:, c, 3:515], in_=pm[:], func=AF.Copy), [pk], ["ubuf"])
                else:
                    gi = c - 4
                    P.op("act", "activation", dict(out=qraw[:], in_=pm[:], func=AF.Copy), [pk], ["qraw"])
                    P.op("act", "activation", dict(out=qsq[:], in_=qraw[:], func=AF.Square), ["qraw"], ["qsq"])
                    pm2, pk2 = nextM()
                    mm(pm2[:], CBF["ones"], qsq[:], True, True, ["cstb", "qsq"], [pk2])
                    P.op("act", "activation", dict(out=qln[:], in_=pm2[:], func=AF.Ln, scale=1.0 / 128.0, bias=EPS),
                         [pk2], ["qrs"])
                    P.op("act", "activation", dict(out=qrs[:], in_=qln[:], func=AF.Exp, scale=-0.5), ["qrs"], ["qrs"])
                    dst = qn[:] if gi == 0 else KT[:, t0:t0 + 512]
                    dk = "qn" if gi == 0 else "KT"
                    P.op("dve", "scalar_tensor_tensor", dict(out=dst, in0=qraw[:], scalar=qk_g[:, gi:gi + 1],
                                                                                  in1=qrs[:], op0=ALU.mult, op1=ALU.mult),
                         ["qraw", "qk_g", "qrs"], [dk])
            if FS < 4:
                return
            for c in range(4):
                P.op("dve", "tensor_scalar", dict(out=cacc[:], in0=ubuf[:, c, 0:512], scalar1=cw[:, 4 * c:4 * c + 1],
                                                           scalar2=cb[:, c:c + 1], op0=ALU.mult, op1=ALU.add),
                     ["ubuf", "cw", "cb"], ["cacc"])
                for k in range(1, 4):
                    P.op("dve", "scalar_tensor_tensor", dict(out=cacc[:], in0=ubuf[:, c, k:k + 512],
                                                                            scalar=cw[:, 4 * c + k:4 * c + k + 1], in1=cacc[:],
                                                                            op0=ALU.mult, op1=ALU.add),
                         ["ubuf", "cw", "cacc"], ["cacc"])
                P.op("act", "activation", dict(out=xbcT[:, c, :], in_=cacc[:], func=AF.Silu), ["cacc"], ["xbcT"])
            P.op("pool", "tensor_copy", dict(out=ubuf[:, :, 0:3], in_=ubuf[:, :, 512:515]), ["ubuf"], ["ubuf"])
            if FS < 5:
                return
            pdt, pdk = nextM()
            for q in range(4):
                for kc in range(8):
                    mm(pdt[:, 64 * q:64 * q + 64], hT[:, kc, 128 * q:128 * (q + 1)], wbf[:, kc, CA_DT:CA_END], kc == 0, kc == 7,
                       ["hT", "wbf"], [pdk])
            P.op("dve", "tensor_copy", dict(out=dt256[:], in_=pdt[:, 0:256]), [pdk], ["dt256"])
            P.op("dve", "tensor_tensor", dict(out=dtt[:].rearrange("p (q r) -> p q r", q=4),
                                                  in0=dt256[:].rearrange("p (q c) -> p q c", q=4)[:, :, 0:4],
                                                  in1=rowb[:, RW_DTB:RW_DTB + 4].unsqueeze(1).to_broadcast([128, 4, 4]),
                                                  op=ALU.add), ["dt256", "rowb"], ["dtt"])
            P.op("act", "activation", dict(out=dte[:], in_=dtt[:], func=AF.Exp), ["dtt"], ["dte"])
            P.op("act", "activation", dict(out=dtv[:], in_=dte[:], func=AF.Ln, bias=1.0), ["dte"], ["dtv"])
            P.op("dve", "tensor_tensor", dict(out=da.rearrange("p (q r) -> p q r", q=4),
                                                  in0=dtv[:].rearrange("p (q r) -> p q r", q=4),
                                                  in1=negA[:].unsqueeze(1).to_broadcast([128, 4, 4]), op=ALU.mult),
                 ["dtv", "negA"], ["da"])
            if FS < 6:
                return
            pm, pk = nextM()
            mm(pm[:, 0:64], C32["LE"], da64[:], True, True, ["cst", "da"], [pk])
            mm(pm[:, 64:128], C32["ones"], da64[:], True, True, ["cst", "da"], [pk])
            P.op("dve", "tensor_copy", dict(out=T48[:, 0:16], in_=pm[:, 0:16]), [pk], ["T48"])
            P.op("dve", "tensor_copy", dict(out=T48[:, 16:32], in_=pm[:, 64:80]), [pk], ["T48"])
            P.op("dve", "tensor_tensor", dict(out=T48[:, 32:48], in0=T48[:, 16:32], in1=T48[:, 0:16], op=ALU.subtract),
                 ["T48"], ["T48"])
            P.op("act", "activation", dict(out=E48[:], in_=T48[:], func=AF.Exp), ["T48"], ["E48"])
            if FS < 7:
                return
            for q in range(4):
                pm, pk = nextM()
                for kc in range(8):
                    mm(pm[:], hT[:, kc, 128 * q:128 * (q + 1)], wbf[:, kc, CA_Z:CA_DT], kc == 0, kc == 7, ["hT", "wbf"], [pk])
                FSX = os.environ.get("FSX", "bcd")
                if "b" in FSX:
                    P.op("act", "activation", dict(out=siluz[:, q, :], in_=pm[:, 0:256], func=AF.Silu), [pk], ["siluz"])
                if "c" in FSX:
                    P.op("act", "activation", dict(out=VA[:, 4 * I + q, :], in_=pm[:, 256:384], func=AF.Copy), [pk], ["VA"])
                if "d" in FSX:
                    P.op("act", "activation", dict(out=utok[:, 1 + q, :], in_=pm[:, 384:512], func=AF.Copy), [pk], ["utok"])

        def ssd(I):
            Y = ystage[I % 2]; Yk = "ystage%d" % (I % 2)
            for q in range(4):
                cs = slice(128 * q, 128 * (q + 1))
                dq = slice(4 * q, 4 * q + 4)
                pm, pk = nextM()
                mm(pm[:, 0:128], xbcT[:, 0, cs], CBF["ident"], True, True, ["xbcT", "cstb"], [pk])
                mm(pm[:, 128:256], xbcT[:, 1, cs], CBF["ident"], True, True, ["xbcT", "cstb"], [pk])
                mm(pm[:, 256:384], xbcT[:, 2, cs], CBF["ident"], True, True, ["xbcT", "cstb"], [pk])
                P.op("act", "activation", dict(out=xtok[:], in_=pm[:, 0:256], func=AF.Copy), [pk], ["xtok"])
                P.op("act", "activation", dict(out=btok[:], in_=pm[:, 256:384], func=AF.Copy), [pk], ["btok"])
                P.op("dve", "tensor_tensor", dict(out=U[:], in0=C32["GT"].unsqueeze(1).to_broadcast([128, 4, 128]),
                                                             in1=da64[:, dq].unsqueeze(2).to_broadcast([128, 4, 128]), op=ALU.mult),
                     ["cst", "da"], ["U"])
                pseg, psk = nextM()
                for r in range(4):
                    mm(pseg[:, 128 * r:128 * (r + 1)], U[:, r, :], C32["LE"], True, True, ["U", "cst"], [psk])
                P.op("act", "activation", dict(out=eseg[:].rearrange("p r l -> p (r l)"), in_=pseg[:], func=AF.Exp),
                     [psk], ["eseg"])
                pcb, pck = nextM()
                mm(pcb[:, 0:128], xbcT[:, 2, cs], xbcT[:, 3, cs], True, True, ["xbcT"], [pck])
                P.op("dve", "tensor_tensor", dict(out=cbm[:], in0=pcb[:, 0:128], in1=C32["LE"], op=ALU.mult),
                     [pck, "cst"], ["cbm"])
                P.op("dve", "tensor_tensor", dict(out=MT[:], in0=eseg[:], in1=cbm[:].unsqueeze(1).to_broadcast([128, 4, 128]),
                                                      op=ALU.mult), ["eseg", "cbm"], ["MT"])
                P.op("dve", "tensor_tensor", dict(out=xdt[:], in0=xtok[:].rearrange("p (r c) -> p r c", r=4),
                                                             in1=dtv[:, dq].unsqueeze(2).to_broadcast([128, 4, 64]), op=ALU.mult),
                     ["xtok", "dtv"], ["xdt"])
                P.op("dve", "tensor_tensor", dict(out=dtte[:], in0=dtv[:, 4 * q:4 * q + 4], in1=E48[:, 32 + 4 * q:36 + 4 * q],
                                                           op=ALU.mult), ["dtv", "E48"], ["dtte"])
                P.op("dve", "tensor_tensor", dict(out=xw[:], in0=xtok[:].rearrange("p (r c) -> p r c", r=4),
                                                      in1=dtte[:].unsqueeze(2).to_broadcast([128, 4, 64]), op=ALU.mult),
                     ["xtok", "dtte"], ["xw"])
                py, pyk = nextM()
                for r in range(4):
                    mm(py[:, 64 * r:64 * (r + 1)], MT[:, r, :], xdt[:, r, :], True, True, ["MT", "xdt"], [pyk])
                mm(py[:, 256:512], xbcT[:, 3, cs], stb[:], True, True, ["xbcT", "stb"], [pyk])
                P.op("dve", "tensor_tensor", dict(out=y1[:].rearrange("p (r c) -> p r c", r=4),
                                                                   in0=py[:, 256:512].rearrange("p (r c) -> p r c", r=4),
                                                                   in1=E48[:, 4 * q:4 * q + 4].unsqueeze(2).to_broadcast([128, 4, 64]),
                                                                   op=ALU.mult), [pyk, "E48"], ["y1"])
                P.op("dve", "tensor_tensor", dict(out=y1[:], in0=y1[:], in1=py[:, 0:256], op=ALU.add), [pyk, "y1"], ["y1"])
                P.op("dve", "tensor_tensor", dict(out=y2[:], in0=xtok[:], in1=rowb[:, RW_DSK:RW_DSK + 256], op=ALU.mult),
                     ["xtok", "rowb"], ["y2"])
                P.op("dve", "tensor_tensor", dict(out=y1[:], in0=y1[:], in1=y2[:], op=ALU.add), ["y1", "y2"], ["y1"])
                pst, pstk = nextM()
                mm(pst[:, 0:256], btok[:], xw[:].rearrange("p r c -> p (r c)"), True, True, ["btok", "xw"], [pstk])
                P.op("dve", "tensor_tensor", dict(out=stT[:].rearrange("p (r c) -> p r c", r=4),
                                                           in0=stT[:].rearrange("p (r c) -> p r c", r=4),
                                                           in1=E48[:, 16 + 4 * q:20 + 4 * q].unsqueeze(2).to_broadcast([128, 4, 64]),
                                                           op=ALU.mult), ["stT", "E48"], ["stT"])
                P.op("dve", "tensor_tensor", dict(out=stT[:], in0=stT[:], in1=pst[:, 0:256], op=ALU.add),
                     ["stT", pstk], ["stT"])
                P.op("act", "activation", dict(out=stb[:], in_=stT[:], func=AF.Copy), ["stT"], ["stb"])
                P.op("dve", "tensor_tensor", dict(out=y1[:], in0=y1[:], in1=siluz[:, q, :], op=ALU.mult),
                     ["y1", "siluz"], ["y1"])
                P.op("act", "activation", dict(out=ysq[:], in_=y1[:], func=AF.Square), ["y1"], ["y2"])
                P.op("dve", "reduce_sum", dict(out=ss[:], in_=ysq[:], axis=AX.X), ["y2"], ["ss"])
                P.op("act", "activation", dict(out=ssl[:], in_=ss[:], func=AF.Ln, scale=1.0 / 256.0, bias=EPS), ["ss"], ["ssl"])
                P.op("act", "activation", dict(out=srs[:], in_=ssl[:], func=AF.Exp, scale=-0.5), ["ssl"], ["srs"])
                P.op("dve", "scalar_tensor_tensor", dict(out=yo[:], in0=y1[:], scalar=srs[:, 0:1], in1=rowb[:, RW_NG:RW_NG + 256],
                                                             op0=ALU.mult, op1=ALU.mult), ["y1", "srs", "rowb"], ["yo"])
                pt, ptk = nextM()
                mm(pt[:, 0:128], yo[:, 0:128], CBF["ident"], True, True, ["yo", "cstb"], [ptk])
                mm(pt[:, 128:256], yo[:, 128:256], CBF["ident"], True, True, ["yo", "cstb"], [ptk])
                P.op("act", "activation", dict(out=Y[:, 0:2, cs],
                                                                 in_=pt[:, 0:256].rearrange("p (h t) -> p h t", h=2), func=AF.Copy),
                     [ptk], [Yk])

        def pool(I):
            Y = ystage[I % 2]; Yk = "ystage%d" % (I % 2)
            pm, pk = nextM()
            for q in range(4):
                first = (I == 0 and q == 0)
                o = pm[:, 128 * q:128 * (q + 1)]
                if first:
                    mm(o, utok[:, 1, :], C32["Pfirst"], True, True, ["utok", "cst"], [pk])
                else:
                    mm(o, utok[:, 1 + q, :], C32["Pmain"], True, False, ["utok", "cst"], [pk])
                    mm(o, utok[:, q, :], C32["Pprev"], False, True, ["utok", "cst"], [pk])
            P.op("act", "activation", dict(out=plT[:], in_=pm[:], func=AF.Copy), [pk], ["plT"])
            P.op("pool", "tensor_copy", dict(out=utok[:, 0, :], in_=utok[:, 4, :]), ["utok"], ["utok"])
            pm2, pk2 = nextM()
            mm(pm2[:], pwb[:], plT[:], True, True, ["pwb", "plT"], [pk2])
            P.op("act", "activation", dict(out=Y[:, 3, :], in_=pm2[:], func=AF.Copy, scale=psc[:, 0:1]),
                 [pk2, "psc"], [Yk])

        def attn(I):
            Y = ystage[I % 2]; Yk = "ystage%d" % (I % 2)
            units = list(range(4 * I + 3, -1, -1))
            nu = len(units)
            P.op("pool", "memset", dict(ap=Rb[:], constant=0.0), [], ["Rb"])
            mm(psO[:], zerob[:], qn[:], True, False, ["zerob", "qn"], ["psO"])

            def rng(kb):
                m = kb - 4 * I
                return (128 * m if m > 0 else 0), m

            def stage1(ui):
                kb = units[ui]; b = ui % 2
                c0, m = rng(kb)
                A = psA[b]; Ak = "psA%d" % b
                mm(A[:, c0:512], KT[:, 128 * kb:128 * (kb + 1)], qn[:, c0:512], True, True, ["KT", "qn"], [Ak])
                P.op("act", "activation", dict(out=ex[b][:, c0:512], in_=A[:, c0:512], func=AF.Exp), [Ak], ["ex%d" % b])
                P.op("act", "activation", dict(out=spb[b][:, c0:512], in_=ex[b][:, c0:512], func=AF.Ln, bias=1.0),
                     ["ex%d" % b], ["spb%d" % b])
                if m >= 0:
                    P.op("pool", "tensor_tensor", dict(out=spb[b][:, c0:c0 + 128], in0=spb[b][:, c0:c0 + 128], in1=CBF["LT"],
                                                           op=ALU.mult), ["spb%d" % b, "cstb"], ["spb%d" % b])

            def stage2(ui):
                kb = units[ui]; b = ui % 2
                c0, m = rng(kb)
                Bp = psB[b]; Bk = "psB%d" % b
                mm(Bp[:, c0:512], KT[:, 128 * kb:128 * (kb + 1)], qn[:, c0:512], True, False, ["KT", "qn"], [Bk])
                mm(Bp[:, c0:512], CBF["negGE"], spb[b][:, c0:512], False, True, ["cstb", "spb%d" % b], [Bk])
                last = (ui == nu - 1)
                if not last:
                    mm(psC[:, c0:512], CBF["ones"], spb[b][:, c0:512], True, True, ["cstb", "spb%d" % b], ["psC"])
                P.op("dve", "tensor_tensor", dict(out=arg[b][:, c0:512], in0=Bp[:, c0:512], in1=Rb[:, c0:512], op=ALU.subtract),
                     [Bk, "Rb"], ["arg%d" % b])
                P.op("act", "activation", dict(out=Wb[b][:, c0:512], in_=arg[b][:, c0:512], func=AF.Exp),
                     ["arg%d" % b], ["Wb%d" % b])
                if m >= 0:
                    P.op("pool", "tensor_tensor", dict(out=Wb[b][:, c0:c0 + 128], in0=Wb[b][:, c0:c0 + 128], in1=CBF["LT"],
                                                           op=ALU.mult), ["Wb%d" % b, "cstb"], ["Wb%d" % b])
                if not last:
                    P.op("dve", "tensor_tensor", dict(out=Rb[:, c0:512], in0=Rb[:, c0:512], in1=psC[:, c0:512], op=ALU.add),
                         ["Rb", "psC"], ["Rb"])

            def stage3(ui):
                kb = units[ui]; b = ui % 2
                c0, m = rng(kb)
                mm(psO[:, c0:512], VA[:, kb, :], Wb[b][:, c0:512], False, ui == nu - 1, ["VA", "Wb%d" % b], ["psO"])

            for s_ in range(nu + 2):
                if s_ < nu:
                    stage1(s_)
                if 0 <= s_ - 1 < nu:
                    stage2(s_ - 1)
                if 0 <= s_ - 2 < nu:
                    stage3(s_ - 2)
            P.op("act", "activation", dict(out=Y[:, 2, :], in_=psO[:], func=AF.Copy), ["psO"], [Yk])

        STG = os.environ.get("STG", "fspa")
        for I in range(NT):
            if "f" in STG:
                front(I)
            if "s" in STG:
                ssd(I)
            if "p" in STG:
                pool(I)
            if "a" in STG:
                attn(I)
            Yk = "ystage%d" % (I % 2)
            P.dma(yT[:, :, 512 * I:512 * (I + 1)].rearrange("c p t -> p c t"), ystage[I % 2][:], reads=[Yk])
        P.finish()
        print("sbuf remaining", nc.sbuf_bytes_remaining); print("build_A ops:", P.n_ops, {k: v for k, v in P.cnt.items()})
    return nc


POOL_WINDOWS = (2, 4, 8, 16)
C_Z0, C_XBC0, C_DT0, C_Q0, C_K0, C_V0, C_U0, C_G0 = 0, 1024, 3072, 3088, 3600, 4112, 4624, 5136


def to_pk(a):
    K, N = a.shape
    return np.ascontiguousarray(a.reshape(K // 128, 128, N).transpose(1, 0, 2))


def prep_A(inp, layer, j):
    w_in = inp["w_in"][layer]
    cols = np.concatenate([
        np.arange(C_XBC0 + 256 * j, C_XBC0 + 256 * j + 256),
        np.arange(C_XBC0 + 1024 + 128 * j, C_XBC0 + 1024 + 128 * j + 128),
        np.arange(C_XBC0 + 1536 + 128 * j, C_XBC0 + 1536 + 128 * j + 128),
        np.arange(C_Q0 + 128 * j, C_Q0 + 128 * j + 128),
        np.arange(C_K0 + 128 * j, C_K0 + 128 * j + 128),
        np.arange(C_Z0 + 256 * j, C_Z0 + 256 * j + 256),
        np.arange(C_V0 + 128 * j, C_V0 + 128 * j + 128),
        np.arange(C_U0 + 128 * j, C_U0 + 128 * j + 128),
        np.arange(C_DT0 + 4 * j, C_DT0 + 4 * j + 4),
    ])
    d = {}
    wsel = np.concatenate([w_in[:, cols], np.zeros((1024, 60), np.float32)], axis=1)
    d["wA"] = to_pk(wsel)
    d["gmix"] = np.ascontiguousarray(inp["mix_norm_g"][layer].reshape(8, 128).T)
    cch = np.concatenate([np.arange(256 * j, 256 * j + 256), np.arange(1024 + 128 * j, 1024 + 128 * j + 128),
                          np.arange(1536 + 128 * j, 1536 + 128 * j + 128)])
    cwl = inp["conv_w"][layer][:, cch]
    d["convw"] = np.ascontiguousarray(cwl.reshape(4, 4, 128).transpose(2, 1, 0).reshape(128, 16))
    d["convb"] = np.ascontiguousarray(inp["conv_b"][layer][cch].reshape(4, 128).T)
    d["rows"] = np.concatenate([
        inp["dt_bias"][layer][4 * j:4 * j + 4], inp["a_log"][layer][4 * j:4 * j + 4],
        np.repeat(inp["d_skip"][layer][4 * j:4 * j + 4], 64),
        inp["ssd_norm_g"][layer][256 * j:256 * j + 256]]).astype(np.float32)[None, :]
    d["qkg"] = np.ascontiguousarray(np.stack([inp["q_norm_g"][layer], inp["k_norm_g"][layer]], axis=1))
    d["poolw"] = np.ascontiguousarray(inp["pool_w"][layer][j])
    d["pscale"] = np.ascontiguousarray(inp["pool_scale"][layer][128 * j:128 * j + 128][:, None])
    d["consts"] = host_consts(POOL_WINDOWS[j])
    return d


def x_to_T(xb):
    S = xb.shape[0]
    return np.ascontiguousarray(xb.T.reshape(8, 128, S).transpose(1, 0, 2))


def build_B(moe, NTOK=4096):
    NT = NTOK // 512
    FF = 1792 if moe else 2816
    NFC = FF // 128
    NE = 8 if moe else 1
    nc = bass.Bass("TRN2", target_bir_lowering=False)

    def din(name, shape, dt=F32):
        return nc.dram_tensor(name, list(shape), dt, kind="ExternalInput").ap()

    xT = din("xT", [128, 8, NTOK])
    yT = din("yT", [128, 16, NTOK], BF16)
    gmix = din("gmix", [128, 8])
    gffn = din("gffn", [128, 8])
    wg = din("wg", [128, 8, 3072])
    wbr = din("wbr", [128, 16, 1024])
    wout = din("wout", [128, 8, 1024])
    consts = din("consts", [128, 2 * 128])
    if moe:
        router = din("router", [128, 8, 64])
        wfg = din("wfg", [8, 128, 8, FF]); wfu = din("wfu", [8, 128, 8, FF]); wfd = din("wfd", [8, 128, NFC, 1024])
    else:
        wfg = din("wfg", [1, 128, 8, FF]); wfu = din("wfu", [1, 128, 8, FF]); wfd = din("wfd", [1, 128, NFC, 1024])
    xo = nc.dram_tensor("xo", [128, 8, NTOK], F32, kind="ExternalOutput").ap()

    with ExitStack() as st:
        P = Prog(nc, st)
        sb, ps = P.sb, P.ps
        cst = sb("cst", [128, 2, 128]); onesb = sb("onesb", [128, 128], BF16)
        ident32 = cst[:, 0, :]; ones32 = cst[:, 1, :]
        gmx = sb("gmx", [128, 8]); gfn = sb("gfn", [128, 8])
        X = sb("X", [128, 8, 512]); Y = sb("Y", [128, 16, 512], BF16)
        sq = sb("sq", [128, 8, 512], BF16)
        rstd = sb("rstd", [128, 512])
        hT = sb("hT", [128, 8, 512], BF16)
        merged = sb("merged", [128, 8, 512], BF16)
        sig = [sb("sig%d" % i, [128, 512]) for i in range(3)]
        tm = [sb("tm0", [128, 512]), sb("tm1", [128, 512])]
        aT = sb("aT", [128, NFC, 512], BF16)
        stage = [sb("stage0", [128, 4096]), sb("stage1", [128, 4096])]
        wbuf = [sb("wbuf%d" % i, [128, 4096], BF16) for i in range(6)]
        if moe:
            h2f = sb("h2f", [128, 8, 512])
            rt = sb("rt", [128, 8, 64])
            lg = sb("lg", [128, 8]); lg2 = sb("lg2", [128, 8]); eq1 = sb("eq1", [128, 8]); eq2 = sb("eq2", [128, 8])
            m1 = sb("m1", [128, 1]); m2 = sb("m2", [128, 1]); dd = sb("dd", [128, 1]); ed = sb("ed", [128, 1])
            w1 = sb("w1", [128, 1]); w2 = sb("w2", [128, 1]); gw = sb("gw", [128, 8])
            gwB = [sb("gwB0", [128, 128]), sb("gwB1", [128, 128])]
            gwb = sb("gwb", [128, 8, 512])
        NPS = 8
        psX = [ps("psX%d" % i, [128, 512]) for i in range(NPS)]
        prr = [0]

        def nextP():
            i = prr[0]
            prr[0] = (i + 1) % NPS
            return psX[i], "psX%d" % i

        def mm(out, lhsT, rhs, start, stop, reads, writes):
            P.op("pe", "matmul", dict(out=out, lhsT=lhsT, rhs=rhs, start=start, stop=stop), reads, writes)

        srr = [0]; wrr = [0]; crr = [0]

        def wblock(src, KC, n):
            s = srr[0]; srr[0] ^= 1
            w = wrr[0]; wrr[0] = (w + 1) % 6
            sk = "stage%d" % s; wk = "wbuf%d" % w
            sv = stage[s][:, 0:KC * n].rearrange("p (k n) -> p k n", k=KC)
            wv = wbuf[w][:, 0:KC * n].rearrange("p (k n) -> p k n", k=KC)
            P.dma(sv, src, writes=[sk])
            c = crr[0]; crr[0] = (c + 1) % 2
            if c == 0:
                P.op("pool", "tensor_copy", dict(out=wv, in_=sv), [sk], [wk])
            else:
                P.op("act", "activation", dict(out=wv, in_=sv, func=AF.Copy), [sk], [wk])
            return wv, wk

        P.dma(cst[:].rearrange("p a b -> p (a b)"), consts, writes=["cst"])
        P.op("dve", "tensor_copy", dict(out=onesb[:], in_=cst[:, 1, :]), ["cst"], ["onesb"])
        P.dma(gmx[:], gmix, writes=["gmx"])
        P.dma(gfn[:], gffn, writes=["gfn"])
        if moe:
            P.dma(rt[:], router, writes=["rt"])

        def norm(gk, g, want_f32):
            P.op("act", "activation", dict(out=sq[:], in_=X[:], func=AF.Square), ["X"], ["sq"])
            pm, pk = nextP()
            for kc in range(8):
                mm(pm[:], onesb[:], sq[:, kc, :], kc == 0, kc == 7, ["onesb", "sq"], [pk])
            P.op("act", "activation", dict(out=rstd[:], in_=pm[:], func=AF.Ln, scale=1.0 / 1024.0, bias=EPS), [pk], ["rstd"])
            P.op("act", "activation", dict(out=rstd[:], in_=rstd[:], func=AF.Exp, scale=-0.5), ["rstd"], ["rstd"])
            for kc in range(8):
                P.op("dve", "scalar_tensor_tensor", dict(out=hT[:, kc, :], in0=X[:, kc, :], scalar=g[:, kc:kc + 1], in1=rstd[:],
                                                         op0=ALU.mult, op1=ALU.mult), ["X", gk, "rstd"], ["hT"])
                if want_f32:
                    P.op("dve", "scalar_tensor_tensor", dict(out=h2f[:, kc, :], in0=X[:, kc, :], scalar=g[:, kc:kc + 1], in1=rstd[:],
                                                             op0=ALU.mult, op1=ALU.mult), ["X", gk, "rstd"], ["h2f"])

        BR_KC = ([4 * j + c for j in range(4) for c in (0, 1)], [4 * j + 2 for j in range(4)], [4 * j + 3 for j in range(4)])

        for I in range(NT):
            ts_ = slice(512 * I, 512 * (I + 1))
            P.dma(X[:], xT[:, :, ts_], writes=["X"])
            P.dma(Y[:], yT[:, :, ts_], writes=["Y"])
            norm("gmx", gmx, False)
            for ocg in range(2):
                gb = [wblock(wg[:, :, i * 1024 + ocg * 512:i * 1024 + (ocg + 1) * 512], 8, 512) for i in range(3)]
                bb = [wblock(wbr[:, :, ocg * 512 + h * 256:ocg * 512 + (h + 1) * 256], 16, 256) for h in range(2)]
                for ocl in range(4):
                    oc = 4 * ocg + ocl
                    for i in range(3):
                        pg, pgk = nextP()
                        for kc in range(8):
                            mm(pg[:], gb[i][0][:, kc, 128 * ocl:128 * (ocl + 1)], hT[:, kc, :], kc == 0, kc == 7, [gb[i][1], "hT"], [pgk])
                        P.op("act", "activation", dict(out=sig[i][:], in_=pg[:], func=AF.Sigmoid), [pgk], ["sig%d" % i])
                    bblk, bk = bb[ocl // 2]
                    bc = slice(128 * (ocl % 2), 128 * (ocl % 2 + 1))
                    pbs = []
                    for i in range(3):
                        pb, pbk = nextP()
                        kcs = BR_KC[i]
                        for n_, kc in enumerate(kcs):
                            mm(pb[:], bblk[:, kc, bc], Y[:, kc, :], n_ == 0, n_ == len(kcs) - 1, [bk, "Y"], [pbk])
                        pbs.append((pb, pbk))
                    P.op("dve", "tensor_tensor", dict(out=tm[0][:], in0=sig[0][:], in1=pbs[0][0][:], op=ALU.mult), ["sig0", pbs[0][1]], ["tm0"])
                    P.op("dve", "tensor_tensor", dict(out=tm[1][:], in0=sig[1][:], in1=pbs[1][0][:], op=ALU.mult), ["sig1", pbs[1][1]], ["tm1"])
                    P.op("dve", "tensor_tensor", dict(out=tm[0][:], in0=tm[0][:], in1=tm[1][:], op=ALU.add), ["tm0", "tm1"], ["tm0"])
                    P.op("dve", "tensor_tensor", dict(out=tm[1][:], in0=sig[2][:], in1=pbs[2][0][:], op=ALU.mult), ["sig2", pbs[2][1]], ["tm1"])
                    P.op("dve", "tensor_tensor", dict(out=merged[:, oc, :], in0=tm[0][:], in1=tm[1][:], op=ALU.add), ["tm0", "tm1"], ["merged"])
            for ocg in range(2):
                ob, obk = wblock(wout[:, :, ocg * 512:(ocg + 1) * 512], 8, 512)
                for ocl in range(4):
                    oc = 4 * ocg + ocl
                    po, pok = nextP()
                    for kc in range(8):
                        mm(po[:], ob[:, kc, 128 * ocl:128 * (ocl + 1)], merged[:, kc, :], kc == 0, kc == 7, [obk, "merged"], [pok])
                    P.op("dve", "tensor_tensor", dict(out=X[:, oc, :], in0=X[:, oc, :], in1=po[:], op=ALU.add), ["X", pok], ["X"])
            norm("gfn", gfn, moe)
            if moe:
                for q in range(4):
                    pr, prk = nextP()
                    for kc in range(8):
                        mm(pr[:, 0:64], h2f[:, kc, 128 * q:128 * (q + 1)], rt[:, kc, :], kc == 0, kc == 7, ["h2f", "rt"], [prk])
                    P.op("dve", "tensor_copy", dict(out=lg[:], in_=pr[:, 0:8]), [prk], ["lg"])
                    P.op("dve", "reduce_max", dict(out=m1[:], in_=lg[:], axis=AX.X), ["lg"], ["m1"])
                    P.op("dve", "tensor_scalar", dict(out=eq1[:], in0=lg[:], scalar1=m1[:, 0:1], scalar2=None, op0=ALU.is_equal), ["lg", "m1"], ["eq1"])
                    P.op("dve", "scalar_tensor_tensor", dict(out=lg2[:], in0=eq1[:], scalar=-1e30, in1=lg[:], op0=ALU.mult, op1=ALU.add),
                         ["eq1", "lg"], ["lg2"])
                    P.op("dve", "reduce_max", dict(out=m2[:], in_=lg2[:], axis=AX.X), ["lg2"], ["m2"])
                    P.op("dve", "tensor_scalar", dict(out=eq2[:], in0=lg2[:], scalar1=m2[:, 0:1], scalar2=None, op0=ALU.is_equal), ["lg2", "m2"], ["eq2"])
                    P.op("dve", "tensor_tensor", dict(out=dd[:], in0=m2[:], in1=m1[:], op=ALU.subtract), ["m1", "m2"], ["dd"])
                    P.op("act", "activation", dict(out=ed[:], in_=dd[:], func=AF.Exp), ["dd"], ["ed"])
                    P.op("dve", "tensor_scalar", dict(out=w1[:], in0=ed[:], scalar1=1.0, scalar2=None, op0=ALU.add), ["ed"], ["w1"])
                    P.op("dve", "reciprocal", dict(out=w1[:], in_=w1[:]), ["w1"], ["w1"])
                    P.op("dve", "tensor_tensor", dict(out=w2[:], in0=ed[:], in1=w1[:], op=ALU.mult), ["ed", "w1"], ["w2"])
                    P.op("dve", "tensor_scalar", dict(out=gw[:], in0=eq1[:], scalar1=w1[:, 0:1], scalar2=None, op0=ALU.mult), ["eq1", "w1"], ["gw"])
                    P.op("dve", "scalar_tensor_tensor", dict(out=gw[:], in0=eq2[:], scalar=w2[:, 0:1], in1=gw[:], op0=ALU.mult, op1=ALU.add),
                         ["eq2", "w2", "gw"], ["gw"])
                    for e_ in range(8):
                        gB = gwB[e_ % 2]; gBk = "gwB%d" % (e_ % 2)
                        P.op("dve", "tensor_scalar", dict(out=gB[:], in0=ones32, scalar1=gw[:, e_:e_ + 1], scalar2=None, op0=ALU.mult),
                             ["cst", "gw"], [gBk])
                        pgw, pgwk = nextP()
                        mm(pgw[:, 0:128], gB[:], ident32, True, True, [gBk, "cst"], [pgwk])
                        P.op("act", "activation", dict(out=gwb[:, e_, 128 * q:128 * (q + 1)], in_=pgw[:, 0:128], func=AF.Copy), [pgwk], ["gwb"])
            for e_ in range(NE):
                ngrp = (FF + 511) // 512
                for fg in range(ngrp):
                    f0 = 512 * fg; f1 = min(FF, f0 + 512); n = f1 - f0
                    gblk, gk_ = wblock(wfg[e_, :, :, f0:f1], 8, n)
                    ublk, uk_ = wblock(wfu[e_, :, :, f0:f1], 8, n)
                    for fl in range(n // 128):
                        fc = 4 * fg + fl
                        pg, pgk = nextP()
                        for kc in range(8):
                            mm(pg[:], gblk[:, kc, 128 * fl:128 * (fl + 1)], hT[:, kc, :], kc == 0, kc == 7, [gk_, "hT"], [pgk])
                        pu, puk = nextP()
                        for kc in range(8):
                            mm(pu[:], ublk[:, kc, 128 * fl:128 * (fl + 1)], hT[:, kc, :], kc == 0, kc == 7, [uk_, "hT"], [puk])
                        P.op("act", "activation", dict(out=sig[fc % 2][:], in_=pg[:], func=AF.Silu), [pgk], ["sig%d" % (fc % 2)])
                        if moe:
                            P.op("dve", "tensor_tensor", dict(out=tm[fc % 2][:], in0=sig[fc % 2][:], in1=pu[:], op=ALU.mult),
                                 ["sig%d" % (fc % 2), puk], ["tm%d" % (fc % 2)])
                            P.op("dve", "tensor_tensor", dict(out=aT[:, fc, :], in0=tm[fc % 2][:], in1=gwb[:, e_, :], op=ALU.mult),
                                 ["tm%d" % (fc % 2), "gwb"], ["aT"])
                        else:
                            P.op("dve", "tensor_tensor", dict(out=aT[:, fc, :], in0=sig[fc % 2][:], in1=pu[:], op=ALU.mult),
                                 ["sig%d" % (fc % 2), puk], ["aT"])
                dn = 4096 // NFC // 128 * 128
                for og in range(1024 // dn):
                    dblk, dk_ = wblock(wfd[e_, :, :, og * dn:(og + 1) * dn], NFC, dn)
                    for ol in range(dn // 128):
                        oc = og * (dn // 128) + ol
                        pd, pdk = nextP()
                        for fc in range(NFC):
                            mm(pd[:], dblk[:, fc, 128 * ol:128 * (ol + 1)], aT[:, fc, :], fc == 0, fc == NFC - 1, [dk_, "aT"], [pdk])
                        P.op("dve", "tensor_tensor", dict(out=X[:, oc, :], in0=X[:, oc, :], in1=pd[:], op=ALU.add), ["X", pdk], ["X"])
            P.dma(xo[:, :, ts_], X[:], reads=["X"])
        P.finish()
        print("build_B ops:", P.n_ops, "sbuf remaining", nc.sbuf_bytes_remaining)
    return nc


def prep_B(inp, layer):
    moe = (layer % 2 == 1)
    d = {}
    d["gmix"] = np.ascontiguousarray(inp["mix_norm_g"][layer].reshape(8, 128).T)
    d["gffn"] = np.ascontiguousarray(inp["ffn_norm_g"][layer].reshape(8, 128).T)
    d["wg"] = to_pk(inp["w_in"][layer][:, C_G0:C_G0 + 3072])
    rows = []
    for j in range(4):
        rows.append(inp["w_br_ssd"][layer][256 * j:256 * j + 128])
        rows.append(inp["w_br_ssd"][layer][256 * j + 128:256 * j + 256])
        rows.append(inp["w_br_sb"][layer][128 * j:128 * j + 128])
        rows.append(inp["w_br_pool"][layer][128 * j:128 * j + 128])
    d["wbr"] = np.ascontiguousarray(np.stack(rows, axis=1))
    d["wout"] = to_pk(inp["w_out"][layer])
    p = np.arange(128)[:, None]; f = np.arange(128)[None, :]
    d["consts"] = np.concatenate([(p == f).astype(np.float32), np.ones((128, 128), np.float32)], axis=1)
    i = layer // 2
    if moe:
        rw = np.concatenate([inp["router_w"][i], np.zeros((1024, 56), np.float32)], axis=1)
        d["router"] = to_pk(rw)
        d["wfg"] = np.stack([to_pk(inp["moe_w_gate"][i][e]) for e in range(8)])
        d["wfu"] = np.stack([to_pk(inp["moe_w_up"][i][e]) for e in range(8)])
        d["wfd"] = np.stack([to_pk(inp["moe_w_down"][i][e]) for e in range(8)])
    else:
        d["wfg"] = to_pk(inp["ffn_w_gate"][i])[None]
        d["wfu"] = to_pk(inp["ffn_w_up"][i])[None]
        d["wfd"] = to_pk(inp["ffn_w_down"][i])[None]
    return d


_PROGS = {}


def _prog(key):
    if key not in _PROGS:
        if key == "A":
            _PROGS[key] = build_A(16384)
        elif key == "Bd":
            _PROGS[key] = build_B(False)
        else:
            _PROGS[key] = build_B(True)
    return _PROGS[key]


def kernel(**inp):
    inp = {k: np.asarray(v) for k, v in inp.items()}
    S = 16384
    xTb = [x_to_T(inp["x"][b]) for b in range(2)]
    for layer in range(2):
        pa = [prep_A(inp, layer, j) for j in range(4)]
        maps = []
        for c in range(8):
            b, j = c // 4, c % 4
            m = dict(pa[j]); m["xT"] = xTb[b]
            maps.append(m)
        resA = run_bass_kernel_spmd(_prog("A"), maps, core_ids=list(range(8))).results
        pb = prep_B(inp, layer)
        maps = []
        for c in range(8):
            b, jj = c // 4, c % 4
            ts_ = slice(4096 * jj, 4096 * (jj + 1))
            yin = np.stack([np.asarray(resA[b * 4 + j]["yT"])[ci][:, ts_] for j in range(4) for ci in range(4)], axis=1)
            m = dict(pb); m["xT"] = np.ascontiguousarray(xTb[b][:, :, ts_]); m["yT"] = np.ascontiguousarray(yin)
            maps.append(m)
        resB = run_bass_kernel_spmd(_prog("Bm" if layer % 2 else "Bd"), maps, core_ids=list(range(8))).results
        for c in range(8):
            b, jj = c // 4, c % 4
            xTb[b][:, :, 4096 * jj:4096 * (jj + 1)] = np.asarray(resB[c]["xo"])
    out = np.stack([np.ascontiguousarray(xTb[b].transpose(1, 0, 2).reshape(1024, S).T) for b in range(2)])
    return out.astype(np.float32)
```
